# Optimizing a Trainium2 kernel written in Bass

```python
import math
import jax, jax.numpy as jnp
from jax import lax
import numpy as np

D_MODEL = 2048
BATCH = 4
SEQ = 2048
DEPTH = 1
DEC_BATCH = 128
DEC_SEQ = 8
PAST_LEN = 16384
PAGE_SIZE = 128

MIX_WIDTH = D_MODEL
CONV_CH = MIX_WIDTH // 2
CONV_KERNEL = 31
GLA_HEADS = 4
GLA_DV = (MIX_WIDTH - CONV_CH) // GLA_HEADS
GLA_DK = GLA_DV // 2
GLA_QK = GLA_HEADS * GLA_DK
GLA_V = GLA_HEADS * GLA_DV
GLA_RANK = 16
GLA_GATE_NORM = 16.0
GLA_CHUNK = 64
PROJ_DIM = 2 * CONV_CH + 2 * GLA_QK + 2 * GLA_V + GLA_RANK
N_EXPERTS = 32
TOP_K = 4
D_FF = D_MODEL
SWIGLU_LIMIT = 7.0
SWIGLU_ALPHA = 1.702
MOE_BLOCK = 128
EPS = 1e-5

kernel_name = "hymba_conformer_gla_moe_step"


def _rmsnorm(x, g):
    xf = x.astype(jnp.float32)
    y = xf * lax.rsqrt(jnp.mean(xf * xf, axis=-1, keepdims=True) + EPS)
    return (y * g.astype(jnp.float32)).astype(x.dtype)


def _layernorm(x, g, b):
    xf = x.astype(jnp.float32)
    mu = jnp.mean(xf, axis=-1, keepdims=True)
    var = jnp.mean(jnp.square(xf - mu), axis=-1, keepdims=True)
    y = (xf - mu) * lax.rsqrt(var + EPS)
    return (y * g.astype(jnp.float32) + b.astype(jnp.float32)).astype(x.dtype)


def _conv_mixer(u_a, u_b, conv_buf, w_dw, b_dw, ln_g, ln_b):
    u = u_a * jax.nn.sigmoid(u_b)
    full = jnp.concatenate([conv_buf.astype(u.dtype), u], axis=1)
    y = lax.conv_general_dilated(full, w_dw[:, None, :].astype(u.dtype), (1,), 'VALID',
                                 dimension_numbers=('NWC', 'WIO', 'NWC'),
                                 feature_group_count=CONV_CH) + b_dw
    new_buf = full[:, -(CONV_KERNEL - 1):, :]
    y = jax.nn.silu(_layernorm(y, ln_g, ln_b))
    return y, new_buf


def _to_chunks(t, n, c):
    b, _, h, d = t.shape
    return t.reshape(b, n, c, h, d).transpose(1, 0, 3, 2, 4)


def _gla_mixer(q, k, v, g, a_low, state0, w_alpha, b_alpha, gn_g):
    bsz, t_len, _ = q.shape
    f32 = jnp.float32
    log_a = jax.nn.log_sigmoid((a_low @ w_alpha + b_alpha).astype(f32)) / GLA_GATE_NORM
    qh = q.astype(f32).reshape(bsz, t_len, GLA_HEADS, GLA_DK) * (GLA_DK ** -0.5)
    kh = k.astype(f32).reshape(bsz, t_len, GLA_HEADS, GLA_DK)
    vh = v.astype(f32).reshape(bsz, t_len, GLA_HEADS, GLA_DV)
    lh = log_a.reshape(bsz, t_len, GLA_HEADS, GLA_DK)
    c = math.gcd(t_len, GLA_CHUNK)
    n = t_len // c
    mask = jnp.tril(jnp.ones((c, c), dtype=bool))

    def step(s, inp):
        qc, kc, vc, lc = inp
        bcum = jnp.cumsum(lc, axis=2)
        o_inter = jnp.einsum('bhik,bhkv->bhiv', qc * jnp.exp(bcum), s)
        diff = jnp.where(mask[:, :, None],
                         bcum[:, :, :, None, :] - bcum[:, :, None, :, :], -jnp.inf)
        att = jnp.einsum('bhik,bhjk,bhijk->bhij', qc, kc, jnp.exp(diff))
        o_intra = jnp.einsum('bhij,bhjv->bhiv', att, vc)
        b_last = bcum[:, :, -1:, :]
        s_new = jnp.exp(b_last[:, :, 0, :])[..., None] * s + jnp.einsum(
            'bhck,bhcv->bhkv', kc * jnp.exp(b_last - bcum), vc)
        return s_new, o_inter + o_intra

    s_fin, o = lax.scan(step, state0.astype(f32),
                        (_to_chunks(qh, n, c), _to_chunks(kh, n, c),
                         _to_chunks(vh, n, c), _to_chunks(lh, n, c)))
    o = o.transpose(1, 0, 3, 2, 4).reshape(bsz, t_len, GLA_HEADS, GLA_DV)
    o = o * lax.rsqrt(jnp.mean(o * o, axis=-1, keepdims=True) + EPS) * gn_g.astype(f32)
    gh = g.astype(f32).reshape(bsz, t_len, GLA_HEADS, GLA_DV)
    o = (o * jax.nn.silu(gh)).reshape(bsz, t_len, GLA_V)
    return o.astype(q.dtype), s_fin


def _moe(h, w_router, b_router, w_gate, b_gate, w_up, b_up, w_down, b_down):
    bsz, t_len, d = h.shape
    xt = h.reshape(-1, d)
    n_tok = xt.shape[0]
    logits = (xt @ w_router + b_router).astype(jnp.float32)
    top_v, top_i = lax.top_k(logits, TOP_K)
    top_w = jax.nn.softmax(top_v, axis=-1)
    n_asg = n_tok * TOP_K
    e_flat = top_i.reshape(-1)
    tok_flat = jnp.repeat(jnp.arange(n_tok, dtype=jnp.int32), TOP_K)
    w_flat = top_w.reshape(-1)
    order = jnp.argsort(e_flat)
    e_s, tok_s, w_s = e_flat[order], tok_flat[order], w_flat[order]
    counts = jnp.bincount(e_flat, length=N_EXPERTS)
    blocks_per = (counts + MOE_BLOCK - 1) // MOE_BLOCK
    blk_end = jnp.cumsum(blocks_per)
    blk_start = blk_end - blocks_per
    grp_start = jnp.cumsum(counts) - counts
    rank = jnp.arange(n_asg) - grp_start[e_s]
    slot = blk_start[e_s] * MOE_BLOCK + rank
    n_blocks = -(-n_asg // MOE_BLOCK) + N_EXPERTS
    n_slots = n_blocks * MOE_BLOCK
    slot_tok = jnp.zeros((n_slots,), jnp.int32).at[slot].set(tok_s)
    slot_w = jnp.zeros((n_slots,), jnp.float32).at[slot].set(w_s)
    blk_expert = jnp.minimum(jnp.searchsorted(blk_end, jnp.arange(n_blocks), side='right'),
                             N_EXPERTS - 1)
    xb = xt[slot_tok].reshape(n_blocks, MOE_BLOCK, d)

    def run(args):
        xblk, e = args
        gate = jnp.minimum(xblk @ w_gate[e] + b_gate[e], SWIGLU_LIMIT)
        up = jnp.clip(xblk @ w_up[e] + b_up[e], -SWIGLU_LIMIT, SWIGLU_LIMIT)
        glu = gate * jax.nn.sigmoid(SWIGLU_ALPHA * gate)
        return ((up + 1.0) * glu) @ w_down[e] + b_down[e]

    yb = lax.map(run, (xb, blk_expert)).reshape(n_slots, d)
    y = jnp.zeros((n_tok, d), jnp.float32).at[slot_tok].add(
        yb.astype(jnp.float32) * slot_w[:, None])
    return y.astype(h.dtype).reshape(bsz, t_len, d)


def _layer(x, c, conv_buf, gla_state, w_ada, b_ada, norm1_g, w_in, w_dw, b_dw,
           conv_ln_g, conv_ln_b, w_alpha, b_alpha, gla_norm_g, w_out, norm2_g,
           w_router, b_router, w_gate, b_gate, w_up, b_up, w_down, b_down):
    mod = jax.nn.silu(c) @ w_ada + b_ada
    sh1, sc1, g1, sh2, sc2, g2 = [m[:, None, :] for m in jnp.split(mod, 6, axis=-1)]
    h = _rmsnorm(x, norm1_g) * (1.0 + sc1) + sh1
    u = h @ w_in
    cuts = np.cumsum([CONV_CH, CONV_CH, GLA_QK, GLA_QK, GLA_V, GLA_V]).tolist()
    u_a, u_b, q, k, v, g, a_low = jnp.split(u, cuts, axis=-1)
    y_conv, new_buf = _conv_mixer(u_a, u_b, conv_buf, w_dw, b_dw, conv_ln_g, conv_ln_b)
    y_gla, new_state = _gla_mixer(q, k, v, g, a_low, gla_state, w_alpha, b_alpha, gla_norm_g)
    x = x + g1 * (jnp.concatenate([y_conv, y_gla], axis=-1) @ w_out)
    h2 = _rmsnorm(x, norm2_g) * (1.0 + sc2) + sh2
    x = x + g2 * _moe(h2, w_router, b_router, w_gate, b_gate, w_up, b_up, w_down, b_down)
    return x, new_buf, new_state


def setup_inputs(seed: int = 0) -> dict:
    key = jax.random.key(seed)
    ks = jax.random.split(key, 32)
    f32 = jnp.float32

    def nrm(k, shape, scale):
        return jax.random.normal(k, shape, f32) * scale

    L, D, E, F = DEPTH, D_MODEL, N_EXPERTS, D_FF
    return {
        "x_prompt": nrm(ks[0], (BATCH, SEQ, D), 1.0),
        "x_sample": nrm(ks[1], (DEC_BATCH, DEC_SEQ, D), 1.0),
        "c_prompt": nrm(ks[2], (BATCH, D), 1.0),
        "c_sample": nrm(ks[3], (DEC_BATCH, D), 1.0),
        "state_conv": nrm(ks[4], (L, DEC_BATCH, CONV_KERNEL - 1, CONV_CH), 0.5),
        "state_gla": nrm(ks[5], (L, DEC_BATCH, GLA_HEADS, GLA_DK, GLA_DV), 0.5),
        "w_ada": nrm(ks[6], (L, D, 6 * D), 0.5 * D ** -0.5),
        "b_ada": nrm(ks[7], (L, 6 * D), 0.02),
        "norm1_g": 1.0 + nrm(ks[8], (L, D), 0.02),
        "w_in": nrm(ks[9], (L, D, PROJ_DIM), D ** -0.5),
        "w_dw": nrm(ks[10], (L, CONV_KERNEL, CONV_CH), CONV_KERNEL ** -0.5),
        "b_dw": nrm(ks[11], (L, CONV_CH), 0.02),
        "conv_ln_g": 1.0 + nrm(ks[12], (L, CONV_CH), 0.02),
        "conv_ln_b": nrm(ks[13], (L, CONV_CH), 0.02),
        "w_alpha": nrm(ks[14], (L, GLA_RANK, GLA_QK), GLA_RANK ** -0.5),
        "b_alpha": nrm(ks[15], (L, GLA_QK), 0.1),
        "gla_norm_g": 1.0 + nrm(ks[16], (L, GLA_DV), 0.02),
        "w_out": nrm(ks[17], (L, MIX_WIDTH, D), MIX_WIDTH ** -0.5),
        "norm2_g": 1.0 + nrm(ks[18], (L, D), 0.02),
        "w_router": nrm(ks[19], (L, D, E), D ** -0.5),
        "b_router": nrm(ks[20], (L, E), 0.01),
        "w_gate": nrm(ks[21], (L, E, D, F), D ** -0.5),
        "b_gate": nrm(ks[22], (L, E, F), 0.02),
        "w_up": nrm(ks[23], (L, E, D, F), D ** -0.5),
        "b_up": nrm(ks[24], (L, E, F), 0.02),
        "w_down": nrm(ks[25], (L, E, F, D), F ** -0.5),
        "b_down": nrm(ks[26], (L, E, D), 0.02),
        "final_norm_g": 1.0 + nrm(ks[27], (D,), 0.02),
    }


def reference(x_prompt, x_sample, c_prompt, c_sample, state_conv, state_gla,
              w_ada, b_ada, norm1_g, w_in, w_dw, b_dw, conv_ln_g, conv_ln_b,
              w_alpha, b_alpha, gla_norm_g, w_out, norm2_g, w_router, b_router,
              w_gate, b_gate, w_up, b_up, w_down, b_down, final_norm_g):
    xp, xs = x_prompt, x_sample
    conv_p, gla_p, conv_s, gla_s = [], [], [], []
    for l in range(DEPTH):
        lw = (w_ada[l], b_ada[l], norm1_g[l], w_in[l], w_dw[l], b_dw[l], conv_ln_g[l],
              conv_ln_b[l], w_alpha[l], b_alpha[l], gla_norm_g[l], w_out[l], norm2_g[l],
              w_router[l], b_router[l], w_gate[l], b_gate[l], w_up[l], b_up[l],
              w_down[l], b_down[l])
        buf0 = jnp.zeros((xp.shape[0], CONV_KERNEL - 1, CONV_CH), xp.dtype)
        st0 = jnp.zeros((xp.shape[0], GLA_HEADS, GLA_DK, GLA_DV), jnp.float32)
        xp, nb_p, ns_p = _layer(xp, c_prompt, buf0, st0, *lw)
        xs, nb_s, ns_s = _layer(xs, c_sample, state_conv[l], state_gla[l], *lw)
        conv_p.append(nb_p)
        gla_p.append(ns_p.astype(x_prompt.dtype))
        conv_s.append(nb_s.astype(state_conv.dtype))
        gla_s.append(ns_s.astype(state_gla.dtype))
    y_prompt = _rmsnorm(xp, final_norm_g)
    y_sample = _rmsnorm(xs, final_norm_g)
    return (y_prompt, y_sample, jnp.stack(conv_p, 0), jnp.stack(gla_p, 0),
            jnp.stack(conv_s, 0), jnp.stack(gla_s, 0))
```

```python
from contextlib import ExitStack
import math
import numpy as np
import concourse.bass as bass
import concourse.mybir as mybir
from concourse.bass_utils import run_bass_kernel_spmd

F32 = mybir.dt.float32
BF16 = mybir.dt.bfloat16
AF = mybir.ActivationFunctionType
ALU = mybir.AluOpType
AX = mybir.AxisListType

NCORES = 8
D = 2048
KC = 16
NT = 9
NTH = 10
TOK = NT * 128
NE = 32
CAP = 384
NSL = CAP // 128
EPS = 1e-5
NMOD = 20


class Op:
    __slots__ = ("eng", "fn", "reads", "writes", "dma", "deps", "sig", "dsem", "dval", "custom", "barrier")

    def __init__(self, eng, fn, reads, writes, dma, custom=None, barrier=False):
        self.eng, self.fn, self.reads, self.writes, self.dma = eng, fn, reads, writes, dma
        self.barrier = barrier
        self.deps = []
        self.sig = None
        self.dsem = None
        self.dval = None
        self.custom = custom


def _key(x):
    if isinstance(x, (str, tuple)):
        return x
    return x.name


class Sched:
    ENGS = ("pe", "act", "dve", "pool", "sp")

    def __init__(self, nc, n_dma_sems=24):
        self.nc = nc
        self.ops = []
        self.n_dma_sems = n_dma_sems

    def op(self, eng, fn, r=(), w=(), dma=False, custom=None):
        self.ops.append(Op(eng, fn, [_key(x) for x in r], [_key(x) for x in w], dma, custom))

    def barrier(self):
        for e in self.ENGS:
            self.ops.append(Op(e, None, [], [], False, None, barrier=True))

    def resolve(self):
        last_w = {}
        readers = {}
        dma_last = [None] * self.n_dma_sems
        dma_cnt = [0] * self.n_dma_sems
        nd = 0
        last_eng = {}
        last_custom = []
        for i, o in enumerate(self.ops):
            deps = set()
            if o.barrier:
                deps = set(last_eng.values()) | set(x for x in dma_last if x is not None) | set(last_custom)
                o.deps = sorted(deps)
                continue
            last_eng[o.eng] = i
            if o.custom is not None:
                last_custom.append(i)
            for k in o.reads:
                if k in last_w:
                    deps.add(last_w[k])
            for k in o.writes:
                if k in last_w:
                    deps.add(last_w[k])
                for j in readers.get(k, ()):
                    deps.add(j)
            if o.dma and o.custom is None:
                s = nd % self.n_dma_sems
                nd += 1
                if dma_last[s] is not None:
                    deps.add(dma_last[s])
                dma_cnt[s] += 1
                o.dsem, o.dval = s, 16 * dma_cnt[s]
                dma_last[s] = i
            deps.discard(i)
            o.deps = sorted(deps)
            for k in o.reads:
                readers.setdefault(k, []).append(i)
            for k in o.writes:
                last_w[k] = i
                readers[k] = []
        need = set()
        for o in self.ops:
            for j in o.deps:
                p = self.ops[j]
                if p.dma:
                    continue
                if p.eng == o.eng and o.eng == "pe" and not o.dma:
                    continue
                need.add(j)
        cnt = {e: 0 for e in self.ENGS}
        for j, o in enumerate(self.ops):
            if j in need:
                cnt[o.eng] += 1
                o.sig = cnt[o.eng]
        self.final_dma = [16 * c for c in dma_cnt]

    def emit(self, es):
        nc = self.nc
        self.resolve()
        sem_e = {e: es.enter_context(nc.semaphore("sem_" + e)) for e in self.ENGS}
        sem_d = [es.enter_context(nc.semaphore("semd%d" % i)) for i in range(self.n_dma_sems)]
        engobj = {"pe": nc.tensor, "act": nc.scalar, "dve": nc.vector, "pool": nc.gpsimd, "sp": nc.sync}
        per_eng = {e: [] for e in self.ENGS}
        for i, o in enumerate(self.ops):
            per_eng[o.eng].append(i)
        ops = self.ops

        def run(ename, eng):
            seen = {}
            for i in per_eng[ename]:
                o = ops[i]
                for j in o.deps:
                    p = ops[j]
                    if p.dma:
                        if p.custom is not None:
                            sem, val = p.custom[0], p.custom[2]
                        else:
                            sem, val = sem_d[p.dsem], p.dval
                    else:
                        if p.eng == ename and ename == "pe" and not o.dma:
                            continue
                        sem, val = sem_e[p.eng], p.sig
                    kk = id(sem)
                    if seen.get(kk, 0) >= val:
                        continue
                    seen[kk] = val
                    eng.wait_ge(sem, val)
                if o.fn is None:
                    continue
                ins = o.fn(eng)
                if o.dma:
                    if o.custom is not None:
                        ins.then_inc(o.custom[0], o.custom[1])
                    else:
                        ins.then_inc(sem_d[o.dsem], 16)
                elif o.sig is not None:
                    ins.then_inc(sem_e[ename], 1)
            if ename == "sp":
                for s, v in zip(sem_d, self.final_dma):
                    if v:
                        eng.wait_ge(s, v)

        with nc.Block() as block:
            @block.tensor
            def _(e):
                run("pe", e)

            @block.scalar
            def _(e):
                run("act", e)

            @block.vector
            def _(e):
                run("dve", e)

            @block.gpsimd
            def _(e):
                run("pool", e)

            @block.sync
            def _(e):
                run("sp", e)


def build_program(with_moe=True, dbg=False, n_exp=NE):
    nc = bass.Bass("TRN2", target_bir_lowering=False)
    es = ExitStack()
    S = Sched(nc)

    def din(name, shape, dt=F32):
        return nc.dram_tensor(name, list(shape), dt, kind="ExternalInput")

    def dout(name, shape, dt=F32):
        return nc.dram_tensor(name, list(shape), dt, kind="ExternalOutput")

    def dint(name, shape, dt=F32):
        return nc.dram_tensor(name, list(shape), dt)

    def sb(name, shape, dt=F32):
        return es.enter_context(nc.sbuf_tensor(name, list(shape), dt))

    xt = din("xt", [NTH, 128, D])
    cc = din("cc", [NMOD, D])
    sconv = din("sconv", [16, 30, 1024])
    sgla = din("sgla", [16, 4, 128, 256])
    ident_d = din("ident", [128, 128])
    maskp_d = din("maskp", [128, 128])
    masks_d = din("masks", [128, 128])
    revp_d = din("revp", [128, 128])
    revs_d = din("revs", [128, 128])
    colmask_d = din("colmask", [128, 16 * 128])
    rowmask_d = din("rowmask", [128, 16])
    selr_d = din("selr", [128, 8])
    hflag_d = din("hflag", [128, 1])
    iotaf_d = din("iotaf", [128, CAP])
    iotap_d = din("iotap", [128, NSL])
    tst_d = din("tst", [128, 128])
    w_ada = din("w_ada", [D, 6 * D])
    b_ada = din("b_ada", [6 * D])
    n1g_d = din("n1g", [128, KC])
    w_in = din("w_in", [D, 5136])
    wdw_d = din("wdw", [128, 8, 31])
    bdw_d = din("bdw", [128, 8])
    clng_d = din("clng", [128, 8])
    clnb_d = din("clnb", [128, 8])
    walpha_d = din("walpha", [17, 512])
    gng_d = din("gng", [256])
    w_out = din("w_out", [D, D])
    n2g_d = din("n2g", [D])
    wr_d = din("wr", [128, KC, NE])
    br_d = din("br", [NE])
    fng_d = din("fng", [D])
    if with_moe:
        w_gate = din("w_gate", [n_exp, D, D])
        w_up = din("w_up", [n_exp, D, D])
        w_down = din("w_down", [n_exp, D, D])
        bg_d = din("bg", [128, NE, KC])
        bu_d = din("bu", [128, NE, KC])
        bd_d = din("bd", [NE, D])

    y_out = dout("y_out", [NT, 128, D])
    convp_out = dout("convp_out", [128, 1024])
    glap_out = dout("glap_out", [4, 4, 128, 256])
    convs_out = dout("convs_out", [16, 30, 1024])
    glas_out = dout("glas_out", [16, 4, 128, 256])

    mod_d = dint("mod_d", [NMOD, 6 * D])
    xmid_d = dint("xmid_d", [NT, 128, D])
    dbgs = {}

    def dbg_out(name, shape, dt=F32):
        if dbg:
            dbgs[name] = dout("dbg_" + name, shape, dt)
            return dbgs[name]
        return None

    NPS = 6
    psf = [es.enter_context(nc.psum_tensor("psf%d" % i, [128, 512], F32)) for i in range(NPS)]
    psb = [es.enter_context(nc.psum_tensor("psb%d" % i, [128, 1024], BF16)) for i in range(2)]
    pctr = [0, 0]

    def PS():
        pctr[0] += 1
        return psf[pctr[0] % NPS]

    def PSB():
        pctr[1] += 1
        return psb[pctr[1] % 2]

    def dma(eng, out, in_, r=(), w=(), **kw):
        S.op(eng, lambda e: e.dma_start(out=out, in_=in_, **kw), r=r, w=w, dma=True)

    def mm(out, lhsT, rhs, start, stop, r=(), w=()):
        S.op("pe", lambda e: e.matmul(out, lhsT, rhs, start=start, stop=stop), r=r, w=w)

    def tr(out, in_, idn, r=(), w=()):
        S.op("pe", lambda e: e.transpose(out, in_, idn), r=r, w=w)

    def act(out, in_, func, r=(), w=(), eng="act", **kw):
        S.op(eng, lambda e: e.activation(out, in_, func, **kw), r=r, w=w)

    def tt(eng, out, a, b, op, r=(), w=()):
        S.op(eng, lambda e: e.tensor_tensor(out, a, b, op), r=r, w=w)

    def ts(eng, out, a, s1, s2, op0, op1=None, r=(), w=()):
        if op1 is None:
            S.op(eng, lambda e: e.tensor_scalar(out, a, s1, None, op0), r=r, w=w)
        else:
            S.op(eng, lambda e: e.tensor_scalar(out, a, s1, s2, op0, op1), r=r, w=w)

    def stt(eng, out, a, s, b, op0, op1, r=(), w=()):
        S.op(eng, lambda e: e.scalar_tensor_tensor(out, a, s, b, op0, op1), r=r, w=w)

    def cp(eng, out, in_, r=(), w=()):
        if eng == "act":
            S.op(eng, lambda e: e.copy(out, in_), r=r, w=w)
        else:
            S.op(eng, lambda e: e.tensor_copy(out, in_), r=r, w=w)

    def mset(eng, ap, v, w=()):
        S.op(eng, lambda e: e.memset(ap, v), w=w)

    def rsqrt_ip(ap, keys, scale, bias):
        act(ap, ap, AF.Sqrt, r=keys, w=keys, scale=scale, bias=bias)
        S.op("dve", lambda e: e.reciprocal(out=ap, in_=ap), r=keys, w=keys)

    ident = sb("identS", [128, 128])
    identb = sb("identB", [128, 128], BF16)
    maskp = sb("maskpS", [128, 128])
    masks = sb("masksS", [128, 128])
    revp = sb("revpS", [128, 128])
    revs = sb("revsS", [128, 128])
    rowmask = sb("rowmaskS", [128, 16])
    selr = sb("selrS", [128, 8])
    hflag = sb("hflagS", [128, 1])
    onesf = sb("onesF", [128, 128])
    n1g = sb("n1gS", [128, KC])
    wdw = sb("wdwS", [128, 8, 31])
    bdw = sb("bdwS", [128, 8])
    clng = sb("clngS", [128, 8])
    clnb = sb("clnbS", [128, 8])
    walpha = sb("walphaS", [17, 512])
    gng = sb("gngS", [128, 256])
    wr = sb("wrS", [128, KC, NE])
    brt = sb("brS", [128, NE])
    for t, d_ in ((ident, ident_d), (maskp, maskp_d), (masks, masks_d), (revp, revp_d), (revs, revs_d),
                  (rowmask, rowmask_d), (selr, selr_d), (hflag, hflag_d), (n1g, n1g_d), (bdw, bdw_d),
                  (clng, clng_d), (clnb, clnb_d), (walpha, walpha_d)):
        dma("sp", t[:], d_.ap(), w=[t])
    dma("sp", wdw[:], wdw_d.ap(), w=[wdw])
    dma("sp", wr[:], wr_d.ap(), w=[wr])
    dma("sp", gng[:], gng_d.ap().partition_broadcast(128), w=[gng])
    dma("sp", brt[:], br_d.ap().partition_broadcast(128), w=[brt])
    cp("dve", identb[:], ident[:], r=[ident], w=[identb])
    mset("dve", onesf[:], 1.0, w=[onesf])

    st1 = ExitStack()

    def sb1(name, shape, dt=F32):
        return st1.enter_context(nc.sbuf_tensor(name, list(shape), dt))

    A1T = sb("A1T", [128, KC, NMOD])
    B1T = sb("B1T", [128, KC, NMOD])
    Gw = sb("Gw", [128, NT, NE])
    maskb = sb("maskb", [128, NT, NE], BF16)
    cS = sb1("cS", [NMOD, D])
    scT = sb1("scT", [128, KC, NMOD], BF16)
    wada_b = [sb1("wada%d" % i, [128, KC, 512], BF16) for i in range(2)]
    modc = [sb1("modc%d" % i, [NMOD, 512]) for i in range(2)]
    badac = [sb1("badac%d" % i, [NMOD, 512]) for i in range(2)]
    n2gc = [sb1("n2gc%d" % i, [NMOD, 512]) for i in range(2)]
    modT = sb1("modT", [128, 2 * KC, NMOD])

    dma("sp", cS[:], cc.ap(), w=[cS])
    act(cS[:], cS[:], AF.Silu, r=[cS], w=[cS])
    p = PS()
    for k in range(KC):
        tr(p[:, k * NMOD:(k + 1) * NMOD], cS[:, k * 128:(k + 1) * 128], ident[0:NMOD, 0:NMOD], r=[cS, ident], w=[p])
    cp("dve", scT[:].rearrange("p a b -> p (a b)"), p[:, 0:KC * NMOD], r=[p], w=[scT])
    w_ada_v = w_ada.ap().rearrange("(k p) n -> p k n", p=128)
    for n in range(24):
        wb = wada_b[n % 2]
        mc = modc[n % 2]
        bc = badac[n % 2]
        dma("pool", wb[:], w_ada_v[:, :, n * 512:(n + 1) * 512], w=[wb])
        dma("sp", bc[:], b_ada.ap()[n * 512:(n + 1) * 512].partition_broadcast(NMOD), w=[bc])
        p = PS()
        for k in range(KC):
            mm(p[0:NMOD, :], scT[:, k, :], wb[:, k, :], k == 0, k == KC - 1, r=[scT, wb], w=[p])
        tt("dve", mc[:], p[0:NMOD, :], bc[:], ALU.add, r=[p, bc], w=[mc])
        sec = n // 4
        if sec == 4:
            g2c = n2gc[n % 2]
            dma("sp", g2c[:], n2g_d.ap()[(n - 16) * 512:(n - 15) * 512].partition_broadcast(NMOD), w=[g2c])
            stt("dve", mc[:], mc[:], 1.0, g2c[:], ALU.add, ALU.mult, r=[mc, g2c], w=[mc])
        dma("sp", mod_d.ap()[:, n * 512:(n + 1) * 512], mc[:], r=[mc], w=["mod_d"])
        if sec < 2:
            p2 = PS()
            for j in range(4):
                tr(p2[:, j * NMOD:(j + 1) * NMOD], mc[:, j * 128:(j + 1) * 128], ident[0:NMOD, 0:NMOD], r=[mc, ident], w=[p2])
            cp("act", modT[:, n * 4:(n + 1) * 4, :].rearrange("p a b -> p (a b)"), p2[:, 0:4 * NMOD], r=[p2], w=[modT])
    cp("dve", B1T[:], modT[:, 0:KC, :], r=[modT], w=[B1T])
    stt("dve", A1T[:], modT[:, KC:2 * KC, :], 1.0, n1g[:].unsqueeze(2).to_broadcast([128, KC, NMOD]),
        ALU.add, ALU.mult, r=[modT, n1g], w=[A1T])
    st1.close()
    S.barrier()

    stM = ExitStack()
    mixT = stM.enter_context(nc.sbuf_tensor("mixT", [128, KC, TOK], BF16))
    st2 = ExitStack()

    def sb2(name, shape, dt=F32):
        return st2.enter_context(nc.sbuf_tensor(name, list(shape), dt))

    hT = sb2("hT", [128, KC, NTH * 128], BF16)
    WC = 256
    win_b = [sb2("winb%d" % i, [128, KC, WC], BF16) for i in range(2)]
    st2a = ExitStack()

    def sb2a(name, shape, dt=F32):
        return st2a.enter_context(nc.sbuf_tensor(name, list(shape), dt))

    xin = [sb2a("xin%d" % i, [128, D]) for i in range(2)]
    xsq = sb2a("xsq", [128, D], BF16)
    xhb = [sb2a("xhb%d" % i, [128, D], BF16) for i in range(2)]
    ssq = [sb2a("ssq%d" % i, [128, 1]) for i in range(2)]
    htmp = sb2a("htmp", [128, 128])

    def rms_rstd(dst, src_ap, junk_ap, keys_r, n):
        mset("dve", dst[:], 0.0, w=[dst])
        act(junk_ap, src_ap, AF.Square, r=list(keys_r) + [dst], w=[junk_ap.name, dst], accum_out=dst[:])
        rsqrt_ip(dst[:], [dst], 1.0 / n, EPS)

    for i in range(NTH):
        xi = xin[i % 2]
        xh = xhb[i % 2]
        sq = ssq[i % 2]
        dma("sp", xi[:], xt.ap()[i], w=[xi])
        rms_rstd(sq, xi[:], xsq[:], [xi], D)
        ts("dve", xh[:], xi[:], sq[:], None, ALU.mult, r=[xi, sq], w=[xh])
        for half in range(2):
            pb = PSB()
            for kk in range(8):
                k = half * 8 + kk
                tr(pb[:, kk * 128:(kk + 1) * 128], xh[:, k * 128:(k + 1) * 128], identb[:], r=[xh, identb], w=[pb])
            for kk in range(8):
                k = half * 8 + kk
                dst = hT[:, k, i * 128:(i + 1) * 128]
                src = pb[:, kk * 128:(kk + 1) * 128]
                if i < 8:
                    b = i // 2
                    if kk % 2 == 0:
                        act(dst, src, AF.Identity, r=[pb, A1T, B1T], w=[("hT", i)],
                            scale=A1T[:, k, b:b + 1], bias=B1T[:, k, b:b + 1])
                    else:
                        ts("dve", dst, src, A1T[:, k, b:b + 1], B1T[:, k, b:b + 1], ALU.mult, ALU.add,
                           r=[pb, A1T, B1T], w=[("hT", i)])
                else:
                    if i == 8:
                        ns, per, m0 = 16, 8, 4
                    else:
                        ns, per, m0 = 4, 32, 0
                    a_b = A1T[:, k, m0:m0 + ns].unsqueeze(2).to_broadcast([128, ns, per])
                    b_b = B1T[:, k, m0:m0 + ns].unsqueeze(2).to_broadcast([128, ns, per])
                    tt("dve", htmp[:].rearrange("p (a b) -> p a b", a=ns), src.rearrange("p (a b) -> p a b", a=ns),
                       a_b, ALU.mult, r=[pb, A1T], w=[htmp])
                    tt("dve", dst.rearrange("p (a b) -> p a b", a=ns), htmp[:].rearrange("p (a b) -> p a b", a=ns),
                       b_b, ALU.add, r=[htmp, B1T], w=[("hT", i)])
    st2a.close()
    S.barrier()
    HT_ALL = [("hT", i) for i in range(NTH)]

    wctr = [0]
    w_in_v = w_in.ap().rearrange("(k p) n -> p k n", p=128)

    def load_win(c0, ncols=WC):
        wctr[0] += 1
        wb = win_b[wctr[0] % 2]
        dma("pool", wb[:, :, 0:ncols], w_in_v[:, :, c0:c0 + ncols], w=[wb])
        return wb

    BLK = [(0, 512), (512, 512), (1024, 128)]
    BLK3 = [(0, 512), (512, 512), (1024, 256)]

    stc = ExitStack()

    def sbc(name, shape, dt=F32):
        return stc.enter_context(nc.sbuf_tensor(name, list(shape), dt))

    UB = 4 * 288 + 16 * 38
    yc = sbc("yc", [128, 8, TOK])
    ubuf = [sbc("ubuf%d" % i, [128, UB]) for i in range(2)]
    sig = sbc("sig", [128, 512])
    usamp = sbc("usamp", [128, 128])
    ycb = sbc("ycb", [128, TOK])
    ycd = sbc("ycd", [128, TOK])
    yct = sbc("yct", [128, TOK])
    scs = sbc("scs", [120, 128])
    ctail = sbc("ctail", [32, 4, 1024])
    cnew = sbc("cnew", [128, 1024])
    dma("sp", convs_out.ap()[:, 0:22, :], sconv.ap()[:, 8:30, :], w=["convs_out_a"])
    for c in range(8):
        wa = load_win(c * 128, 128)
        wu = load_win(1024 + c * 128, 128)
        ub = ubuf[c % 2]
        for bi, (c0, n) in enumerate(BLK3):
            pa_ = PS()
            pu_ = PS()
            for k in range(KC):
                mm(pa_[:, 0:n], wa[:, k, 0:128], hT[:, k, c0:c0 + n], k == 0, k == KC - 1, r=[wa] + HT_ALL, w=[pa_])
            for k in range(KC):
                mm(pu_[:, 0:n], wu[:, k, 0:128], hT[:, k, c0:c0 + n], k == 0, k == KC - 1, r=[wu] + HT_ALL, w=[pu_])
            act(sig[:, 0:n], pu_[:, 0:n], AF.Sigmoid, r=[pu_], w=[sig])
            if bi < 2:
                dst = ub[:, bi * 576:(bi + 1) * 576].rearrange("p (b x) -> p b x", b=2)[:, :, 32:288]
                tt("dve", dst, pa_[:, 0:512].rearrange("p (b x) -> p b x", b=2),
                   sig[:, 0:512].rearrange("p (b x) -> p b x", b=2), ALU.mult, r=[pa_, sig], w=[ub])
            else:
                tt("dve", usamp[:], pa_[:, 0:128], sig[:, 0:128], ALU.mult, r=[pa_, sig], w=[usamp])
                dst = ub[:, 1152:UB].rearrange("p (s x) -> p s x", s=16)[:, :, 30:38]
                cp("dve", dst, usamp[:].rearrange("p (s x) -> p s x", s=16), r=[usamp], w=[ub])
                dsth = ub[:, 0:1152].rearrange("p (b x) -> p b x", b=4)[:, :, 0:32]
                stt("dve", dsth, pa_[:, 128:256].rearrange("p (b x) -> p b x", b=4), hflag[:],
                    sig[:, 128:256].rearrange("p (b x) -> p b x", b=4), ALU.mult, ALU.mult, r=[pa_, sig, hflag], w=[ub])
        for g4 in range(4):
            dma("sp", scs[:], sconv.ap()[g4 * 4:(g4 + 1) * 4, :, c * 128:(c + 1) * 128].rearrange("s r c -> (s r) c"), w=[scs])
            pt_ = PS()
            tr(pt_[:, 0:120], scs[:], ident[0:120, 0:120], r=[scs, ident], w=[pt_])
            dsts = ub[:, 1152 + g4 * 152:1152 + (g4 + 1) * 152].rearrange("p (s x) -> p s x", s=4)[:, :, 0:30]
            cp("act", dsts, pt_[:, 0:120].rearrange("p (s r) -> p s r", s=4), r=[pt_], w=[ub])
        ycp = yc[:, c, 0:1024].rearrange("p (b x) -> p b x", b=4)
        ycs = yc[:, c, 1024:1152].rearrange("p (s x) -> p s x", s=16)
        ybp = ycb[:, 0:1024].rearrange("p (b x) -> p b x", b=4)
        ybs = ycb[:, 1024:1152].rearrange("p (s x) -> p s x", s=16)

        def up(j, ub=ub):
            return ub[:, 0:1152].rearrange("p (b x) -> p b x", b=4)[:, :, 2 + j:2 + j + 256]

        def us(j, ub=ub):
            return ub[:, 1152:UB].rearrange("p (s x) -> p s x", s=16)[:, :, j:j + 8]
        NDVE = 25
        NA = 13
        ytp = yct[:, 0:1024].rearrange("p (b x) -> p b x", b=4)
        yts = yct[:, 1024:1152].rearrange("p (s x) -> p s x", s=16)
        ydp = ycd[:, 0:1024].rearrange("p (b x) -> p b x", b=4)
        yds = ycd[:, 1024:1152].rearrange("p (s x) -> p s x", s=16)
        order = []
        for q in range(NA):
            order.append(q)
            if NA + q < NDVE:
                order.append(NA + q)
        for j in order:
            wj = wdw[:, c, j:j + 1]
            if j < NA:
                key, op_, os_ = ("yc", c), ycp, ycs
            else:
                key, op_, os_ = ycd, ydp, yds
            if j == 0:
                ts("dve", op_, up(j), wj, bdw[:, c:c + 1], ALU.mult, ALU.add, r=[ub, wdw, bdw], w=[key])
                ts("dve", os_, us(j), wj, bdw[:, c:c + 1], ALU.mult, ALU.add, r=[ub, wdw, bdw], w=[key])
            elif j == NA:
                ts("dve", op_, up(j), wj, None, ALU.mult, r=[ub, wdw], w=[key])
                ts("dve", os_, us(j), wj, None, ALU.mult, r=[ub, wdw], w=[key])
            else:
                stt("dve", op_, up(j), wj, op_, ALU.mult, ALU.add, r=[ub, wdw, key], w=[key])
                stt("dve", os_, us(j), wj, os_, ALU.mult, ALU.add, r=[ub, wdw, key], w=[key])
        for j in range(NDVE, 31):
            wj = wdw[:, c, j:j + 1]
            if j == NDVE:
                ts("pool", ybp, up(j), wj, None, ALU.mult, r=[ub, wdw], w=[ycb])
                ts("pool", ybs, us(j), wj, None, ALU.mult, r=[ub, wdw], w=[ycb])
            else:
                ts("pool", ytp, up(j), wj, None, ALU.mult, r=[ub, wdw], w=[yct])
                ts("pool", yts, us(j), wj, None, ALU.mult, r=[ub, wdw], w=[yct])
                tt("pool", ycb[:], ycb[:], yct[:], ALU.add, r=[ycb, yct], w=[ycb])
        tt("dve", yc[:, c, :], yc[:, c, :], ycd[:], ALU.add, r=[("yc", c), ycd], w=[("yc", c)])
        tt("dve", yc[:, c, :], yc[:, c, :], ycb[:], ALU.add, r=[("yc", c), ycb], w=[("yc", c)])
        pt_ = PS()
        for b in range(4):
            tr(pt_[0:32, b * 128:(b + 1) * 128], ub[:, 288 * b + 256:288 * b + 288], ident[:], r=[ub, ident], w=[pt_])
        cp("act", ctail[:, :, c * 128:(c + 1) * 128], pt_[0:32, :].rearrange("p (b x) -> p b x", b=4), r=[pt_], w=[ctail])
        pt2 = PS()
        tr(pt2[:, 0:128], usamp[:], ident[:], r=[usamp, ident], w=[pt2])
        cp("act", cnew[:, c * 128:(c + 1) * 128], pt2[:, 0:128], r=[pt2], w=[cnew])
    dma("sp", convp_out.ap().rearrange("(b r) c -> r b c", b=4), ctail[:], r=[ctail], w=["convp_out"])
    for s in range(16):
        dma("sp", convs_out.ap()[s, 22:30, :], cnew[s * 8:(s + 1) * 8, :], r=[cnew], w=["convs_out_b"])
    mrow = sbc("mrow", [128, TOK])
    rrow = sbc("rrow", [128, TOK])
    ysq = sbc("ysq", [128, 512])
    zt = sbc("zt", [128, 512])
    YC_ALL = [("yc", c) for c in range(8)]
    for (c0, n) in BLK:
        p1 = PS()
        for c in range(8):
            mm(p1[:, 0:n], onesf[:], yc[:, c, c0:c0 + n], c == 0, c == 7, r=[onesf] + YC_ALL, w=[p1])
        p2 = PS()
        for c in range(8):
            act(ysq[:, 0:n], yc[:, c, c0:c0 + n], AF.Square, r=YC_ALL, w=[ysq])
            mm(p2[:, 0:n], onesf[:], ysq[:, 0:n], c == 0, c == 7, r=[onesf, ysq], w=[p2])
        ts("dve", mrow[:, c0:c0 + n], p1[:, 0:n], 1.0 / 1024.0, None, ALU.mult, r=[p1], w=[mrow])
        ts("dve", rrow[:, c0:c0 + n], p2[:, 0:n], 1.0 / 1024.0, EPS, ALU.mult, ALU.add, r=[p2], w=[rrow])
        tt("dve", ysq[:, 0:n], mrow[:, c0:c0 + n], mrow[:, c0:c0 + n], ALU.mult, r=[mrow], w=[ysq])
        tt("dve", rrow[:, c0:c0 + n], rrow[:, c0:c0 + n], ysq[:, 0:n], ALU.subtract, r=[rrow, ysq], w=[rrow])
        rsqrt_ip(rrow[:, c0:c0 + n], [rrow], 1.0, 0.0)
        for c in range(8):
            tt("dve", zt[:, 0:n], yc[:, c, c0:c0 + n], mrow[:, c0:c0 + n], ALU.subtract, r=YC_ALL + [mrow], w=[zt])
            tt("dve", zt[:, 0:n], zt[:, 0:n], rrow[:, c0:c0 + n], ALU.mult, r=[zt, rrow], w=[zt])
            act(mixT[:, c, c0:c0 + n], zt[:, 0:n], AF.Silu, r=[zt, clng, clnb], w=[("mixT", "c")],
                scale=clng[:, c:c + 1], bias=clnb[:, c:c + 1])
    stc.close()
    S.barrier()

    st3 = ExitStack()

    def sb3(name, shape, dt=F32):
        return st3.enter_context(nc.sbuf_tensor(name, list(shape), dt))

    alT = sb3("alT", [17, TOK])
    la = sb3("la", [128, NT, 512])
    etmp = [sb3("etmp%d" % i, [128, 256]) for i in range(2)]
    ectr = [0]

    def ET():
        ectr[0] += 1
        return etmp[ectr[0] % 2]

    wb = load_win(5120, 16)
    mset("dve", alT[:], 1.0, w=[alT])
    for (c0, n) in BLK:
        p = PS()
        for k in range(KC):
            mm(p[0:16, 0:n], wb[:, k, 0:16], hT[:, k, c0:c0 + n], k == 0, k == KC - 1, r=[wb] + HT_ALL, w=[p])
        cp("act", alT[0:16, c0:c0 + n], p[0:16, 0:n], r=[p], w=[alT])
    for i in range(NT):
        p = PS()
        mm(p[:, :], alT[:, i * 128:(i + 1) * 128], walpha[:], True, True, r=[alT, walpha], w=[p])
        for hf in range(2):
            e1 = ET()
            act(e1[:], p[:, hf * 256:(hf + 1) * 256], AF.Exp, r=[p], w=[e1], scale=-1.0)
            act(e1[:], e1[:], AF.Ln, r=[e1], w=[e1], bias=1.0)
            ts("dve", la[:, i, hf * 256:(hf + 1) * 256], e1[:], -1.0 / 16.0, None, ALU.mult, r=[e1], w=[("la", i)])

    lnsc_t = sb3("lnsc", [128, 1])
    mset("dve", lnsc_t[:], math.log(128.0 ** -0.5), w=[lnsc_t])
    ksT = sb3("ksT", [128, NT, 256], BF16)
    qsT = sb3("qsT", [128, NT, 256], BF16)
    khat = sb3("khat", [128, NT, 256], BF16)
    vtok = sb3("vtok", [128, NT, 512], BF16)
    gact = sb3("gact", [128, NT, 512], BF16)
    Dp = sb3("Dp", [128, 8, 2])
    Ds = sb3("Ds", [128, 2, 16])
    Sst = sb3("Sst", [128, 8, 256])
    Sbf = sb3("Sbf", [128, 8, 256], BF16)
    xch = [sb3("xch%d" % i, [128, 257]) for i in range(2)]
    agb = [sb3("agb%d" % i, [128, 2 * 257]) for i in range(2)]
    coef = sb3("coef", [128, 2])
    stmp = sb3("stmp", [128, 256])
    atm = [sb3("atm%d" % i, [128, 256], BF16) for i in range(2)]
    ygla = [sb3("ygla%d" % i, [128, 512], BF16) for i in range(2)]
    osq = sb3("osq", [128, 256], BF16)
    ot1 = sb3("ot1", [128, 256])
    orr = [sb3("orr%d" % i, [128, 1]) for i in range(2)]
    colmask = sb3("colmaskS", [128, 16, 128], BF16)
    qm = sb3("qm", [128, 16, 128], BF16)
    vm = sb3("vm", [128, 256], BF16)
    s0f = [sb3("s0f%d" % i, [128, 256]) for i in range(2)]
    s0b = [sb3("s0b%d" % i, [128, 256], BF16) for i in range(2)]
    snew = [sb3("snew%d" % i, [128, 256]) for i in range(2)]
    dma("pool", colmask[:].rearrange("p a b -> p (a b)"), colmask_d.ap(), w=[colmask])
    mset("pool", qm[:], 0.0, w=[qm])
    qm_diag = bass.AP(qm, 0, [[16 * 128, 128], [136, 16], [1, 8]])
    XW2 = 8 * 257
    for hg in range(2):
        G = "g%d" % hg

        def bcumT_bank(i, hg=hg):
            p = PS()
            mk = maskp if i < 8 else masks
            for hh in range(2):
                h = hg * 2 + hh
                mm(p[:, hh * 128:(hh + 1) * 128], la[:, i, h * 128:(h + 1) * 128], mk[:], True, True,
                   r=[("la", i), mk], w=[p])
            return p
        wb = load_win(2560 + hg * 256)
        for i in range(NT):
            pk = PS()
            for hh in range(2):
                for k in range(KC):
                    mm(pk[:, hh * 128:(hh + 1) * 128], wb[:, k, hh * 128:(hh + 1) * 128], hT[:, k, i * 128:(i + 1) * 128],
                       k == 0, k == KC - 1, r=[wb] + HT_ALL, w=[pk])
            pbk = bcumT_bank(i)
            e2 = ET()
            act(e2[:], pbk[:, 0:256], AF.Exp, r=[pbk], w=[e2], scale=-1.0)
            tt("dve", ksT[:, i, :], pk[:, 0:256], e2[:], ALU.mult, r=[pk, e2], w=[("ksT", G, i)])
            if i < 8:
                act(Dp[:, i, :], pbk[:, 0:256].rearrange("p (h t) -> p h t", h=2)[:, :, 127], AF.Exp, r=[pbk], w=[("Dp", G, i)])
            else:
                act(Ds[:], pbk[:, 0:256].rearrange("p (h s t) -> p h s t", h=2, s=16)[:, :, :, 7], AF.Exp, r=[pbk], w=[("Ds", G)])
            pk2 = PS()
            for k in range(KC):
                mm(pk2[:, 0:256], hT[:, k, i * 128:(i + 1) * 128], wb[:, k, :], k == 0, k == KC - 1, r=[wb] + HT_ALL, w=[pk2])
            pr = PS()
            mm(pr[:, 0:256], (revp if i < 8 else revs)[:], la[:, i, hg * 256:(hg + 1) * 256], True, True,
               r=[("la", i), revp, revs], w=[pr])
            e3 = ET()
            act(e3[:], pr[:, 0:256], AF.Exp, r=[pr], w=[e3])
            tt("dve", khat[:, i, :], pk2[:, 0:256], e3[:], ALU.mult, r=[pk2, e3], w=[("khat", G, i)])
        for hh in range(2):
            wb = load_win(3072 + hg * 512 + hh * 256)
            for i in range(NT):
                p = PS()
                for k in range(KC):
                    mm(p[:, 0:256], hT[:, k, i * 128:(i + 1) * 128], wb[:, k, :], k == 0, k == KC - 1, r=[wb] + HT_ALL, w=[p])
                cp("act", vtok[:, i, hh * 256:(hh + 1) * 256], p[:, 0:256], r=[p], w=[("vtok", G, i, hh)])
        ag_in = dint("ag_in%d" % hg, [128, XW2])
        ag_out = dint("ag_out%d" % hg, [NCORES * 128, XW2])
        for b in range(4):
            for hh in range(2):
                bh = b * 2 + hh
                xc = xch[bh % 2]
                p0 = PS()
                mm(p0[:, 0:256], khat[:, 2 * b, hh * 128:(hh + 1) * 128], vtok[:, 2 * b, hh * 256:(hh + 1) * 256], True, True,
                   r=[("khat", G, 2 * b), ("vtok", G, 2 * b, hh)], w=[p0])
                mm(p0[:, 256:512], khat[:, 2 * b + 1, hh * 128:(hh + 1) * 128], vtok[:, 2 * b + 1, hh * 256:(hh + 1) * 256], True, True,
                   r=[("khat", G, 2 * b + 1), ("vtok", G, 2 * b + 1, hh)], w=[p0])
                ts("dve", stmp[:], p0[:, 0:256], Dp[:, 2 * b + 1, hh:hh + 1], None, ALU.mult, r=[p0, ("Dp", G, 2 * b + 1)], w=[stmp])
                tt("dve", xc[:, 0:256], stmp[:], p0[:, 256:512], ALU.add, r=[stmp, p0], w=[xc])
                tt("dve", xc[:, 256:257], Dp[:, 2 * b, hh:hh + 1], Dp[:, 2 * b + 1, hh:hh + 1], ALU.mult,
                   r=[("Dp", G, 2 * b), ("Dp", G, 2 * b + 1), xc], w=[xc])
                dma("sp", ag_in.ap()[:, bh * 257:(bh + 1) * 257], xc[:], r=[xc], w=["ag_in" + G])
        cc_sem = es.enter_context(nc.semaphore("cc_sem%d" % hg))
        S.op("pool", lambda e, ag_in=ag_in, ag_out=ag_out: e.collective_compute(
            "AllGather", ALU.bypass, replica_groups=[list(range(NCORES))],
            ins=[ag_in.ap().opt()], outs=[ag_out.ap().opt()]),
            r=["ag_in" + G], w=["ag_out" + G], dma=True, custom=(cc_sem, 1, 1))
        ag_v = ag_out.ap().rearrange("(r p) (b x) -> b r p x", p=128, b=4)
        for b in range(4):
            for r_ in range(8):
                ab = agb[(b * 8 + r_) % 2]
                dma("sp", ab[:], ag_v[b, r_], r=["ag_out" + G], w=[ab])
                stt("dve", coef[:], ab[:].rearrange("p (h x) -> p h x", h=2)[:, :, 256], -1.0,
                    selr[:, r_:r_ + 1].to_broadcast([128, 2]), ALU.add, ALU.mult, r=[ab, selr], w=[coef])
                ts("dve", coef[:], coef[:], 1.0, None, ALU.add, r=[coef], w=[coef])
                for hh in range(2):
                    bh = b * 2 + hh
                    if r_ == 0:
                        ts("dve", Sst[:, bh, :], ab[:, hh * 257:hh * 257 + 256], selr[:, r_:r_ + 1], None, ALU.mult,
                           r=[ab, selr], w=[("Sst", G, bh)])
                    else:
                        ts("dve", Sst[:, bh, :], Sst[:, bh, :], coef[:, hh:hh + 1], None, ALU.mult,
                           r=[("Sst", G, bh), coef], w=[("Sst", G, bh)])
                        stt("dve", Sst[:, bh, :], ab[:, hh * 257:hh * 257 + 256], selr[:, r_:r_ + 1], Sst[:, bh, :],
                            ALU.mult, ALU.add, r=[ab, selr, ("Sst", G, bh)], w=[("Sst", G, bh)])
            for hh in range(2):
                bh = b * 2 + hh
                cp("act", Sbf[:, bh, :], Sst[:, bh, :], r=[("Sst", G, bh)], w=[("Sbf", G, bh)])
        wb = load_win(2048 + hg * 256)
        for i in range(NT):
            pq = PS()
            for hh in range(2):
                for k in range(KC):
                    mm(pq[:, hh * 128:(hh + 1) * 128], wb[:, k, hh * 128:(hh + 1) * 128], hT[:, k, i * 128:(i + 1) * 128],
                       k == 0, k == KC - 1, r=[wb] + HT_ALL, w=[pq])
            pbk = bcumT_bank(i)
            e1 = ET()
            act(e1[:], pbk[:, 0:256], AF.Exp, r=[pbk, lnsc_t], w=[e1], bias=lnsc_t[:])
            tt("dve", qsT[:, i, :], pq[:, 0:256], e1[:], ALU.mult, r=[pq, e1], w=[("qsT", G, i)])
        for hh in range(2):
            wb = load_win(4096 + hg * 512 + hh * 256)
            for i in range(NT):
                p = PS()
                for k in range(KC):
                    mm(p[:, 0:256], hT[:, k, i * 128:(i + 1) * 128], wb[:, k, :], k == 0, k == KC - 1, r=[wb] + HT_ALL, w=[p])
                act(gact[:, i, hh * 256:(hh + 1) * 256], p[:, 0:256], AF.Silu, r=[p], w=[("gact", G, i, hh)])
        for i in range(NT):
            b = i // 2
            mk = maskp if i < 8 else masks
            pa = PS()
            for hh in range(2):
                mm(pa[:, hh * 128:(hh + 1) * 128], ksT[:, i, hh * 128:(hh + 1) * 128], qsT[:, i, hh * 128:(hh + 1) * 128], True, True,
                   r=[("ksT", G, i), ("qsT", G, i)], w=[pa])
            am = atm[i % 2]
            tt("dve", am[:].rearrange("p (h t) -> p h t", h=2), pa[:, 0:256].rearrange("p (h t) -> p h t", h=2),
               mk[:].unsqueeze(1).to_broadcast([128, 2, 128]), ALU.mult, r=[pa, mk], w=[am])
            yg = ygla[i % 2]
            for hh in range(2):
                h = hg * 2 + hh
                bh = b * 2 + hh
                po = PS()
                VK = [("vtok", G, i, hh)]
                if i == 8:
                    cp("dve", qm_diag, qsT[:, i, hh * 128:(hh + 1) * 128].rearrange("p (s t) -> p s t", s=16),
                       r=[("qsT", G, i)], w=[qm])
                mm(po[:, 0:256], am[:, hh * 128:(hh + 1) * 128], vtok[:, i, hh * 256:(hh + 1) * 256], True, False,
                   r=[am] + VK, w=[po])
                if i < 8:
                    mm(po[:, 0:256], qsT[:, i, hh * 128:(hh + 1) * 128], Sbf[:, bh, :], False, True,
                       r=[("qsT", G, i), ("Sbf", G, bh)], w=[po])
                    mm(po[:, 256:512], khat[:, i, hh * 128:(hh + 1) * 128], vtok[:, i, hh * 256:(hh + 1) * 256], True, True,
                       r=[("khat", G, i)] + VK, w=[po])
                    stt("dve", Sst[:, bh, :], Sst[:, bh, :], Dp[:, i, hh:hh + 1], po[:, 256:512], ALU.mult, ALU.add,
                        r=[("Sst", G, bh), ("Dp", G, i), po], w=[("Sst", G, bh)])
                    if i % 2 == 0:
                        cp("act", Sbf[:, bh, :], Sst[:, bh, :], r=[("Sst", G, bh)], w=[("Sbf", G, bh)])
                    else:
                        dma("sp", glap_out.ap()[b, h], Sst[:, bh, :], r=[("Sst", G, bh)], w=["glap_out"])
                else:
                    for s in range(16):
                        sbb = s0b[s % 2]
                        dma("pool", sbb[:], sgla.ap()[s, h], w=[sbb])
                        mm(po[:, 0:256], qm[:, s, :], sbb[:], False, s == 15, r=[qm, sbb], w=[po])
                rr = orr[hh]
                if dbg and i == 8 and hg == 0 and hh == 0:
                    d_o = dbg_out("osamp", [128, 256])
                    d_qm = dbg_out("qm", [128, 16 * 128], BF16)
                    d_am = dbg_out("am", [128, 256], BF16)
                    dsb = sb3("dsb", [128, 256])
                    cp("dve", dsb[:], po[:, 0:256], r=[po], w=[dsb])
                    dma("sp", d_o.ap(), dsb[:], r=[dsb], w=["d_o"])
                    dma("sp", d_qm.ap(), qm[:].rearrange("p a b -> p (a b)"), r=[qm], w=["d_qm"])
                    dma("sp", d_am.ap(), am[:], r=[am], w=["d_am"])
                mset("dve", rr[:], 0.0, w=[rr])
                act(osq[:], po[:, 0:256], AF.Square, r=[po, rr], w=[osq, rr], accum_out=rr[:])
                rsqrt_ip(rr[:], [rr], 1.0 / 256.0, EPS)
                stt("dve", ot1[:], po[:, 0:256], rr[:], gng[:], ALU.mult, ALU.mult, r=[po, rr, gng], w=[ot1])
                tt("dve", yg[:, hh * 256:(hh + 1) * 256], ot1[:], gact[:, i, hh * 256:(hh + 1) * 256], ALU.mult,
                   r=[ot1, ("gact", G, i, hh)], w=[yg])
                if i == 8:
                    for s in range(16):
                        sf = s0f[s % 2]
                        dma("sp", sf[:], sgla.ap()[s, h], w=[sf])
                        ts("dve", vm[:], vtok[:, i, hh * 256:(hh + 1) * 256], rowmask[:, s:s + 1], None, ALU.mult,
                           r=VK + [rowmask], w=[vm])
                        pd = PS()
                        mm(pd[:, 0:256], khat[:, i, hh * 128:(hh + 1) * 128], vm[:], True, True, r=[("khat", G, i), vm], w=[pd])
                        sn = snew[s % 2]
                        stt("dve", sn[:], sf[:], Ds[:, hh, s:s + 1], pd[:, 0:256], ALU.mult, ALU.add,
                            r=[sf, ("Ds", G), pd], w=[sn])
                        dma("sp", glas_out.ap()[s, h], sn[:], r=[sn], w=["glas_out"])
            pb = PSB()
            for c in range(4):
                tr(pb[:, c * 128:(c + 1) * 128], yg[:, c * 128:(c + 1) * 128], identb[:], r=[yg, identb], w=[pb])
            cp("act", mixT[:, 8 + hg * 4:12 + hg * 4, i * 128:(i + 1) * 128], pb[:, 0:512].rearrange("p (c t) -> p c t", c=4),
               r=[pb], w=[("mixT", "g", hg, i)])
    st3.close()
    st2.close()
    S.barrier()

    st4 = ExitStack()

    def sb4(name, shape, dt=F32):
        return st4.enter_context(nc.sbuf_tensor(name, list(shape), dt))

    h2_d = dint("h2_d", [NT, 128, D], BF16)
    wo = sb4("wo", [128, KC, D], BF16)
    xm = [sb4("xm%d" % i, [128, D]) for i in range(2)]
    g1t = sb4("g1t", [128, D])
    otmp = [sb4("otmp%d" % i, [128, 512]) for i in range(2)]
    a2t = sb4("a2t", [128, D])
    b2t = sb4("b2t", [128, D])
    h2f = sb4("h2f", [128, D])
    h2b = [sb4("h2b%d" % i, [128, D], BF16) for i in range(2)]
    h2T = sb4("h2T", [128, KC, 128])
    junk = sb4("junk", [128, D], BF16)
    rs2 = [sb4("rs2_%d" % i, [128, 1]) for i in range(2)]
    lg = sb4("lg", [128, NE])
    top8 = sb4("top8", [128, 8])
    nmx = sb4("nmx", [128, 1])
    ex = sb4("ex", [128, NE])
    msk = sb4("msk", [128, NE])
    ssum = sb4("ssum", [128, 1])
    w_out_v = w_out.ap().rearrange("(k p) n -> p k n", p=128)
    MIX_ALL = [("mixT", "c")] + [("mixT", "g", hg, i) for hg in range(2) for i in range(NT)]

    def mod_bcast(dst_ap, i, sec, c0, n, keys_w):
        col = sec * D + c0
        if i < 8:
            dma("sp", dst_ap, mod_d.ap()[i // 2, col:col + n].partition_broadcast(128), r=["mod_d"], w=keys_w)
        else:
            for s in range(16):
                dma("sp", dst_ap[s * 8:(s + 1) * 8, :], mod_d.ap()[4 + s, col:col + n].partition_broadcast(8),
                    r=["mod_d"], w=keys_w)

    for n4 in range(4):
        dma("pool", wo[:, :, n4 * 512:(n4 + 1) * 512], w_out_v[:, :, n4 * 512:(n4 + 1) * 512], w=[("wo", n4)])
    dbg_xmid = dbg_out("xmid", [NT, 128, D])
    dbg_lg = dbg_out("lg", [NT, 128, NE])
    dbg_mix = dbg_out("mix", [128, KC, TOK], ) if False else None
    for i in range(NT):
        x_ = xm[i % 2]
        dma("sp", x_[:], xt.ap()[i], w=[x_])
        mod_bcast(g1t[:], i, 2, 0, D, [g1t])
        for n4 in range(4):
            p = PS()
            for k in range(KC):
                mm(p[:, :], mixT[:, k, i * 128:(i + 1) * 128], wo[:, k, n4 * 512:(n4 + 1) * 512], k == 0, k == KC - 1,
                   r=[("wo", n4)] + MIX_ALL, w=[p])
            ot = otmp[n4 % 2]
            tt("dve", ot[:], p[:, :], g1t[:, n4 * 512:(n4 + 1) * 512], ALU.mult, r=[p, g1t], w=[ot])
            tt("pool", x_[:, n4 * 512:(n4 + 1) * 512], x_[:, n4 * 512:(n4 + 1) * 512], ot[:], ALU.add, r=[ot, x_], w=[x_])
        rs = rs2[i % 2]
        rms_rstd(rs, x_[:], junk[:], [x_], D)
        mod_bcast(a2t[:], i, 4, 0, D, [a2t])
        mod_bcast(b2t[:], i, 3, 0, D, [b2t])
        stt("dve", h2f[:], x_[:], rs[:], a2t[:], ALU.mult, ALU.mult, r=[x_, rs, a2t], w=[h2f])
        tt("dve", h2f[:], h2f[:], b2t[:], ALU.add, r=[h2f, b2t], w=[h2f])
        hb = h2b[i % 2]
        cp("act", hb[:], h2f[:], r=[h2f], w=[hb])
        dma("sp", h2_d.ap()[i], hb[:], r=[hb], w=[("h2_d", i)])
        dma("sp", xmid_d.ap()[i], x_[:], r=[x_], w=[("xmid_d", i)])
        if dbg:
            dma("sp", dbg_xmid.ap()[i], x_[:], r=[x_], w=["dbgx"])
        for q in range(4):
            p = PS()
            for kk in range(4):
                k = q * 4 + kk
                tr(p[:, kk * 128:(kk + 1) * 128], h2f[:, k * 128:(k + 1) * 128], ident[:], r=[h2f, ident], w=[p])
            cp("act" if q % 2 else "dve", h2T[:, q * 4:(q + 1) * 4, :].rearrange("p a b -> p (a b)"), p[:, :], r=[p], w=[h2T])
        p = PS()
        for k in range(KC):
            mm(p[:, 0:NE], h2T[:, k, :], wr[:, k, :], k == 0, k == KC - 1, r=[h2T, wr], w=[p])
        tt("dve", lg[:], p[:, 0:NE], brt[:], ALU.add, r=[p, brt], w=[lg])
        if dbg:
            dma("sp", dbg_lg.ap()[i], lg[:], r=[lg], w=["dbgl"])
        S.op("dve", lambda e: e.max(out=top8[:], in_=lg[:]), r=[lg], w=[top8])
        ts("dve", msk[:], lg[:], top8[:, 3:4], None, ALU.is_ge, r=[lg, top8], w=[msk])
        ts("dve", nmx[:], top8[:, 0:1], -1.0, None, ALU.mult, r=[top8], w=[nmx])
        act(ex[:], lg[:], AF.Exp, r=[lg, nmx], w=[ex], bias=nmx[:])
        tt("dve", ex[:], ex[:], msk[:], ALU.mult, r=[ex, msk], w=[ex])
        S.op("dve", lambda e: e.reduce_sum(out=ssum[:], in_=ex[:], axis=AX.X), r=[ex], w=[ssum])
        S.op("dve", lambda e: e.reciprocal(out=ssum[:], in_=ssum[:]), r=[ssum], w=[ssum])
        ts("dve", Gw[:, i, :], ex[:], ssum[:], None, ALU.mult, r=[ex, ssum], w=[("Gw", i)])
        cp("dve", maskb[:, i, :], msk[:], r=[msk], w=[("maskb", i)])
    st4.close()
    stM.close()
    S.barrier()

    st5 = ExitStack()

    def sb5(name, shape, dt=F32):
        return st5.enter_context(nc.sbuf_tensor(name, list(shape), dt))

    yacc = sb5("yacc", [128, NT, D])
    if with_moe:
        h2bf = sb5("h2bf", [128, NT, D], BF16)
        posm = sb5("posm", [128, NT, NE])
        onesb = sb5("onesb", [128, 128], BF16)
        tstb = sb5("tstb", [128, 128], BF16)
        iotaf = sb5("iotafS", [128, CAP])
        iotap = sb5("iotapS", [128, NSL])
        bgs = sb5("bgs", [128, NE, KC])
        bus = sb5("bus", [128, NE, KC])
        for i in range(NT):
            dma("sp", h2bf[:, i, :], h2_d.ap()[i], r=[("h2_d", i)], w=[("h2bf", i)])
        dma("sp", iotaf[:], iotaf_d.ap(), w=[iotaf])
        dma("sp", iotap[:], iotap_d.ap(), w=[iotap])
        dma("sp", bgs[:], bg_d.ap(), w=[bgs])
        dma("sp", bus[:], bu_d.ap(), w=[bus])
        dma("pool", tstb[:], tst_d.ap(), w=[tstb])
        mset("dve", onesb[:], 1.0, w=[onesb])
        for i in range(NT):
            p = PS()
            for i2 in range(i):
                mm(p[:, 0:NE], onesb[:], maskb[:, i2, :], i2 == 0, False, r=[onesb, ("maskb", i2)], w=[p])
            mm(p[:, 0:NE], tstb[:], maskb[:, i, :], i == 0, True, r=[tstb, ("maskb", i)], w=[p])
            stt("dve", posm[:, i, :], p[:, 0:NE], 1.0, maskb[:, i, :], ALU.add, ALU.mult, r=[p, ("maskb", i)], w=[("posm", i)])
            ts("dve", posm[:, i, :], posm[:, i, :], -1.0, None, ALU.add, r=[("posm", i)], w=[("posm", i)])
        st5a = ExitStack()
        bds = st5a.enter_context(nc.sbuf_tensor("bds", [NE, D], F32))
        GwT = st5a.enter_context(nc.sbuf_tensor("GwT", [NE, NT, 128], F32))
        dma("sp", bds[:], bd_d.ap(), w=[bds])
        for i in range(NT):
            p = PS()
            tr(p[0:NE, 0:128], Gw[:, i, :], ident[:], r=[("Gw", i), ident], w=[p])
            cp("act", GwT[:, i, :], p[0:NE, 0:128], r=[p], w=[("GwT", i)])
            for n4 in range(4):
                p = PS()
                mm(p[:, :], GwT[:, i, :], bds[:, n4 * 512:(n4 + 1) * 512], True, True, r=[("GwT", i), bds], w=[p])
                cp("act" if n4 % 2 else "dve", yacc[:, i, n4 * 512:(n4 + 1) * 512], p[:, :], r=[p], w=[("yacc", i, n4)])
        st5a.close()
        S.barrier()
        st5b = ExitStack()

        def sb5b(name, shape, dt=F32):
            return st5b.enter_context(nc.sbuf_tensor(name, list(shape), dt))
        NR = 4
        RW = 256
        ring = [sb5b("ring%d" % i, [128, KC, RW], BF16) for i in range(NR)]
        Pe = sb5b("Pm", [128, NT, CAP], BF16)
        PTe = sb5b("PTm", [128, NSL, TOK], BF16)
        XeT = sb5b("XeT", [128, KC, CAP], BF16)
        HT = sb5b("HT", [128, KC, CAP], BF16)
        Yb = [sb5b("Yb%d" % i, [128, NSL, RW], BF16) for i in range(2)]
        g1s_ = [sb5b("g1s%d" % i, [128, CAP]) for i in range(2)]
        u1s_ = [sb5b("u1s%d" % i, [128, CAP]) for i in range(2)]
        NQ = D // RW
        wlist = []
        for e in range(n_exp):
            for fq in range(NQ):
                wlist.append((w_gate, e, fq))
                wlist.append((w_up, e, fq))
            for nq in range(NQ):
                wlist.append((w_down, e, nq))
        widx = [0]

        def issue_w(j):
            if j < len(wlist):
                wt, e_, q_ = wlist[j]
                rb = ring[j % NR]
                dma("pool", rb[:], wt.ap()[e_].rearrange("(k p) n -> p k n", p=128)[:, :, q_ * RW:(q_ + 1) * RW], w=[rb])

        for j0 in range(NR):
            issue_w(j0)

        def next_w():
            j = widx[0]
            widx[0] += 1
            return j, ring[j % NR]

        def done_w(j):
            issue_w(j + NR)

        for e in range(n_exp):
            for i in range(NT):
                ts("dve", Pe[:, i, :], iotaf[:], posm[:, i, e:e + 1], None, ALU.is_equal, r=[iotaf, ("posm", i)], w=[("Pe", i)])
            for g3 in range(3):
                p = PS()
                for ii in range(3):
                    i = g3 * 3 + ii
                    mm(p[:, ii * 128:(ii + 1) * 128], posm[:, i, e:e + 1].to_broadcast([128, 128]), ident[:], True, True,
                       r=[("posm", i), ident], w=[p])
                for k3 in range(NSL):
                    ts("dve", PTe[:, k3, g3 * 384:(g3 + 1) * 384], p[:, 0:384], iotap[:, k3:k3 + 1], None, ALU.is_equal,
                       r=[p, iotap], w=[("PTe", g3)])
            PE_ALL = [("Pe", i) for i in range(NT)]
            PT_ALL = [("PTe", g) for g in range(3)]
            for k in range(KC):
                p = PS()
                for i in range(NT):
                    mm(p[:, 0:CAP], h2bf[:, i, k * 128:(k + 1) * 128], Pe[:, i, :], i == 0, i == NT - 1,
                       r=[("h2bf", i)] + PE_ALL, w=[p])
                cp("act", XeT[:, k, :], p[:, 0:CAP], r=[p], w=[("XeT", k)])
            XE_ALL = [("XeT", k) for k in range(KC)]
            for fq in range(NQ):
                jg, wg = next_w()
                ju, wu = next_w()
                for f2 in range(RW // 128):
                    f = fq * (RW // 128) + f2
                    pg = PS()
                    pu = PS()
                    for k in range(KC):
                        mm(pg[:, 0:CAP], wg[:, k, f2 * 128:(f2 + 1) * 128], XeT[:, k, :], k == 0, k == KC - 1, r=[wg] + XE_ALL, w=[pg])
                    for k in range(KC):
                        mm(pu[:, 0:CAP], wu[:, k, f2 * 128:(f2 + 1) * 128], XeT[:, k, :], k == 0, k == KC - 1, r=[wu] + XE_ALL, w=[pu])
                    g1s, u1s = g1s_[f % 2], u1s_[f % 2]
                    ts("dve", g1s[:], pg[:, 0:CAP], bgs[:, e, f:f + 1], 7.0, ALU.add, ALU.min, r=[pg, bgs], w=[g1s])
                    act(g1s[:], g1s[:], AF.Silu, r=[g1s], w=[g1s], scale=1.702)
                    ts("dve", u1s[:], pu[:, 0:CAP], bus[:, e, f:f + 1], 7.0, ALU.add, ALU.min, r=[pu, bus], w=[u1s])
                    ts("dve", u1s[:], u1s[:], -7.0, 1.0, ALU.max, ALU.add, r=[u1s], w=[u1s])
                    stt("dve", HT[:, f, :], g1s[:], 1.0 / 1.702, u1s[:], ALU.mult, ALU.mult, r=[u1s, g1s], w=[("HT", f)])
                done_w(jg)
                done_w(ju)
            H_ALL = [("HT", f) for f in range(KC)]
            for nq in range(NQ):
                jd, wd = next_w()
                yb = Yb[nq % 2]
                for k3 in range(NSL):
                    p = PS()
                    for f in range(KC):
                        mm(p[:, 0:RW], HT[:, f, k3 * 128:(k3 + 1) * 128], wd[:, f, :], f == 0, f == KC - 1, r=[wd] + H_ALL, w=[p])
                    cp("act", yb[:, k3, :], p[:, 0:RW], r=[p], w=[yb])
                done_w(jd)
                for i in range(NT):
                    p = PS()
                    for k3 in range(NSL):
                        mm(p[:, 0:RW], PTe[:, k3, i * 128:(i + 1) * 128], yb[:, k3, :], k3 == 0, k3 == NSL - 1,
                           r=PT_ALL + [yb], w=[p])
                    n4 = (nq * RW) // 512
                    stt("dve", yacc[:, i, nq * RW:(nq + 1) * RW], p[:, 0:RW], Gw[:, i, e:e + 1], yacc[:, i, nq * RW:(nq + 1) * RW],
                        ALU.mult, ALU.add, r=[p, ("Gw", i), ("yacc", i, n4)], w=[("yacc", i, n4)])
        st5b.close()
        S.barrier()
    else:
        for i in range(NT):
            for n4 in range(4):
                mset("dve", yacc[:, i, n4 * 512:(n4 + 1) * 512], 0.0, w=[("yacc", i, n4)])

    if dbg:
        d_ya = dbg_out("yacc", [NT, 128, D])
        for i in range(NT):
            dma("sp", d_ya.ap()[i], yacc[:, i, :], r=[("yacc", i, q) for q in range(4)], w=["d_ya"])
        d_gw = dbg_out("gw", [128, NT, NE])
        dma("sp", d_gw.ap(), Gw[:], r=[("Gw", i) for i in range(NT)], w=["d_gw"])
    xf = [sb5("xf%d" % i, [128, D]) for i in range(2)]
    g2t = sb5("g2t", [128, D])
    fng = sb5("fngS", [128, D])
    junk2 = sb5("junk2", [128, D], BF16)
    rs3 = [sb5("rs3_%d" % i, [128, 1]) for i in range(2)]
    dma("sp", fng[:], fng_d.ap().partition_broadcast(128), w=[fng])
    for i in range(NT):
        x_ = xf[i % 2]
        YA = [("yacc", i, q) for q in range(4)]
        dma("sp", x_[:], xmid_d.ap()[i], r=[("xmid_d", i)], w=[x_])
        mod_bcast(g2t[:], i, 5, 0, D, [g2t])
        tt("dve", yacc[:, i, :], yacc[:, i, :], g2t[:], ALU.mult, r=YA + [g2t], w=YA)
        tt("pool", x_[:], x_[:], yacc[:, i, :], ALU.add, r=[x_] + YA, w=[x_])
        rs = rs3[i % 2]
        rms_rstd(rs, x_[:], junk2[:], [x_], D)
        stt("dve", x_[:], x_[:], rs[:], fng[:], ALU.mult, ALU.mult, r=[x_, rs, fng], w=[x_])
        dma("sp", y_out.ap()[i], x_[:], r=[x_], w=["y_out"])
    st5.close()

    S.emit(es)
    es.close()
    return nc


def _consts():
    j = np.arange(128)
    maskp = (j[:, None] <= j[None, :]).astype(np.float32)
    same8 = (j[:, None] // 8 == j[None, :] // 8)
    masks = (maskp > 0) & same8
    revp = (j[:, None] > j[None, :]).astype(np.float32)
    revs = ((j[:, None] > j[None, :]) & same8).astype(np.float32)
    colmask = np.zeros((16, 128), np.float32)
    for s in range(16):
        colmask[s, s * 8:(s + 1) * 8] = 1.0
    colmask = np.broadcast_to(colmask.reshape(1, 16 * 128), (128, 16 * 128)).copy()
    rowmask = (j[:, None] // 8 == np.arange(16)[None, :]).astype(np.float32)
    return dict(ident=np.eye(128, dtype=np.float32), maskp=maskp, masks=masks.astype(np.float32), revp=revp, revs=revs,
                colmask=colmask, rowmask=rowmask,
                iotaf=np.broadcast_to(np.arange(CAP, dtype=np.float32)[None, :], (128, CAP)).copy(),
                iotap=(j[:, None] + 128 * np.arange(NSL)[None, :]).astype(np.float32),
                tst=(j[:, None] < j[None, :]).astype(np.float32))


def _fm(v, nch):
    return np.ascontiguousarray(np.asarray(v, np.float32).reshape(nch, 128).T)


_CACHE = {}


def kernel(x_prompt, x_sample, c_prompt, c_sample, state_conv, state_gla,
           w_ada, b_ada, norm1_g, w_in, w_dw, b_dw, conv_ln_g, conv_ln_b,
           w_alpha, b_alpha, gla_norm_g, w_out, norm2_g, w_router, b_router,
           w_gate, b_gate, w_up, b_up, w_down, b_down, final_norm_g, _with_moe=True, _dbg=False, _n_exp=NE):
    f = lambda a: np.ascontiguousarray(np.asarray(a, dtype=np.float32))
    x_prompt, x_sample = f(x_prompt), f(x_sample)
    c_prompt, c_sample = f(c_prompt), f(c_sample)
    state_conv, state_gla = f(state_conv)[0], f(state_gla)[0]
    key = (_with_moe, _dbg, _n_exp)
    if key not in _CACHE:
        _CACHE[key] = build_program(_with_moe, _dbg, _n_exp)
    nc = _CACHE[key]
    cst = _consts()
    shared = dict(cst)
    shared.update(
        w_ada=f(w_ada)[0], b_ada=f(b_ada)[0], n1g=_fm(f(norm1_g)[0], KC), w_in=f(w_in)[0],
        wdw=np.ascontiguousarray(f(w_dw)[0].T.reshape(8, 128, 31).transpose(1, 0, 2)),
        bdw=_fm(f(b_dw)[0], 8), clng=_fm(f(conv_ln_g)[0], 8), clnb=_fm(f(conv_ln_b)[0], 8),
        walpha=np.ascontiguousarray(np.concatenate([f(w_alpha)[0], f(b_alpha)[0][None, :]], axis=0)),
        gng=f(gla_norm_g)[0], w_out=f(w_out)[0], n2g=f(norm2_g)[0],
        wr=np.ascontiguousarray(f(w_router)[0].reshape(KC, 128, NE).transpose(1, 0, 2)),
        br=f(b_router)[0], fng=f(final_norm_g))
    if _with_moe:
        shared.update(
            w_gate=f(np.asarray(w_gate)[0, :_n_exp]), w_up=f(np.asarray(w_up)[0, :_n_exp]), w_down=f(np.asarray(w_down)[0, :_n_exp]),
            bg=np.ascontiguousarray(f(b_gate)[0].reshape(NE, KC, 128).transpose(2, 0, 1)),
            bu=np.ascontiguousarray(f(b_up)[0].reshape(NE, KC, 128).transpose(2, 0, 1)),
            bd=f(b_down)[0])
    in_maps = []
    for c in range(NCORES):
        xt = np.zeros((NTH, 128, D), np.float32)
        for b in range(4):
            xt[2 * b] = x_prompt[b, 256 * c:256 * c + 128]
            xt[2 * b + 1] = x_prompt[b, 256 * c + 128:256 * c + 256]
            if c > 0:
                xt[9, 32 * b:32 * b + 32] = x_prompt[b, 256 * c - 32:256 * c]
        xt[8] = x_sample[16 * c:16 * c + 16].reshape(128, D)
        m = dict(shared)
        m.update(
            xt=xt, cc=np.ascontiguousarray(np.concatenate([c_prompt, c_sample[16 * c:16 * c + 16]], axis=0)),
            sconv=np.ascontiguousarray(state_conv[16 * c:16 * c + 16]),
            sgla=np.ascontiguousarray(state_gla[16 * c:16 * c + 16]),
            selr=np.broadcast_to((np.arange(8) < c).astype(np.float32)[None, :], (128, 8)).copy(),
            hflag=np.full((128, 1), 1.0 if c > 0 else 0.0, np.float32))
        in_maps.append(m)
    res = run_bass_kernel_spmd(nc, in_maps, core_ids=list(range(NCORES)))
    R = res.results
    y_prompt = np.zeros((4, 2048, D), np.float32)
    y_sample = np.zeros((128, 8, D), np.float32)
    for c in range(NCORES):
        yo = R[c]["y_out"]
        for b in range(4):
            y_prompt[b, 256 * c:256 * c + 128] = yo[2 * b]
            y_prompt[b, 256 * c + 128:256 * c + 256] = yo[2 * b + 1]
        y_sample[16 * c:16 * c + 16] = yo[8].reshape(16, 8, D)
    conv_p = np.ascontiguousarray(R[7]["convp_out"].reshape(4, 32, 1024)[:, 2:32, :])[None]
    gla_p = np.ascontiguousarray(R[7]["glap_out"])[None]
    conv_s = np.concatenate([R[c]["convs_out"] for c in range(NCORES)], axis=0)[None]
    gla_s = np.concatenate([R[c]["glas_out"] for c in range(NCORES)], axis=0)[None]
    out = (y_prompt, y_sample, conv_p.astype(np.float32), gla_p.astype(np.float32),
           conv_s.astype(np.float32), gla_s.astype(np.float32))
    if _dbg:
        return out, R
    return out
```
